# Optimizing a Trainium2 kernel written in Bass

```python
import jax
import jax.numpy as jnp
from jax import lax
import numpy as np

D_MODEL = 4096
BATCH = 2
SEQ = 8192
DEPTH = 4

CTX_LEN = 256
GRID_W = 64
N_MOD = 6
ADA_RANK = 256
HEAD_DIM = 128
MLA_HEADS = 3 * D_MODEL // (8 * HEAD_DIM)
NA_HEADS = 3 * D_MODEL // (8 * HEAD_DIM)
CONV_CH = D_MODEL - (MLA_HEADS + NA_HEADS) * HEAD_DIM
MLA_Q_RANK = D_MODEL // 4
MLA_KV_RANK = D_MODEL // 8
MLA_NOPE = HEAD_DIM
MLA_ROPE = 64
MLA_V = HEAD_DIM
MLA_QK = MLA_NOPE + MLA_ROPE
ROPE_F = MLA_ROPE // 4
ROPE_BASE = 10000.0
CONV_W = 3
NA_DIM = HEAD_DIM
NA_WIN_R = 8
NA_WIN_C = 16
N_EXPERTS = 16
CAP_FACTOR = 2
EXPERT_FF = 768
Q_BLOCK = 128
EPS = 1e-6
MLA_SCALE = MLA_QK ** -0.5
NA_SCALE = NA_DIM ** -0.5

OFF_CQ = 0
OFF_CKV = OFF_CQ + MLA_Q_RANK
OFF_KR = OFF_CKV + MLA_KV_RANK
OFF_CB = OFF_KR + MLA_ROPE
OFF_CC = OFF_CB + CONV_CH
OFF_CX = OFF_CC + CONV_CH
OFF_NQ = OFF_CX + CONV_CH
OFF_NK = OFF_NQ + NA_HEADS * NA_DIM
OFF_NV = OFF_NK + NA_HEADS * NA_DIM
D_IN = OFF_NV + NA_HEADS * NA_DIM

kernel_name = "hybrid_mla_conv_natten_ecmoe_dit"


def rms_norm(x, g):
    xf = x.astype(jnp.float32)
    y = xf * lax.rsqrt(jnp.mean(xf * xf, axis=-1, keepdims=True) + EPS)
    return (y * g.astype(jnp.float32)).astype(x.dtype)


def ada_mod(cond, down, up, bias):
    m = (jax.nn.silu(cond) @ down) @ up + bias
    return jnp.split(m, N_MOD, axis=-1)


def modulate(h, g, shift, scale):
    return rms_norm(h, g) * (1 + scale) + shift


def axial_rope_tables(n):
    pos = jnp.arange(n, dtype=jnp.int32)
    row = (pos // GRID_W).astype(jnp.float32)
    col = (pos % GRID_W).astype(jnp.float32)
    inv = jnp.power(ROPE_BASE, -jnp.arange(ROPE_F, dtype=jnp.float32) / ROPE_F)
    ang = jnp.stack([row[:, None] * inv, col[:, None] * inv], axis=1)
    return jnp.cos(ang), jnp.sin(ang)


def apply_axial_rope(x, cos, sin):
    xs = x.reshape(x.shape[:-1] + (2, 2, ROPE_F)).astype(jnp.float32)
    x1, x2 = xs[..., 0, :], xs[..., 1, :]
    cs, sn = cos[:, None], sin[:, None]
    out = jnp.stack([x1 * cs - x2 * sn, x2 * cs + x1 * sn], axis=-2)
    return out.reshape(x.shape).astype(x.dtype)


def rope_tail(t, rope):
    if rope is None:
        return t
    cos, sin = rope
    return jnp.concatenate([t[..., :MLA_NOPE], apply_axial_rope(t[..., MLA_NOPE:], cos, sin)], axis=-1)


def mla_q(p_cq, g_qa, w_uq, g_q, rope):
    q = (rms_norm(p_cq, g_qa) @ w_uq).reshape(p_cq.shape[:-1] + (MLA_HEADS, MLA_QK))
    return rope_tail(rms_norm(q, g_q), rope)


def mla_kv(p, g_kva, w_ukv, g_k, rope):
    ckv = rms_norm(p[..., :MLA_KV_RANK], g_kva)
    kv = (ckv @ w_ukv).reshape(p.shape[:-1] + (MLA_HEADS, MLA_NOPE + MLA_V))
    k_nope, v = kv[..., :MLA_NOPE], kv[..., MLA_NOPE:]
    k_rope = jnp.broadcast_to(p[..., None, MLA_KV_RANK:], k_nope.shape[:-1] + (MLA_ROPE,))
    k = rms_norm(jnp.concatenate([k_nope, k_rope], axis=-1), g_k)
    return rope_tail(k, rope), v


def na_q(p, g_q):
    return rms_norm(p.reshape(p.shape[:-1] + (NA_HEADS, NA_DIM)), g_q)


def na_kv(p, g_k):
    hd = NA_HEADS * NA_DIM
    k = rms_norm(p[..., :hd].reshape(p.shape[:-1] + (NA_HEADS, NA_DIM)), g_k)
    v = p[..., hd:].reshape(p.shape[:-1] + (NA_HEADS, NA_DIM))
    return k, v


def dense_attention(q, k, v, scale):
    b, n, h, dq = q.shape
    nb = n // Q_BLOCK
    qb = q.reshape(b, nb, Q_BLOCK, h, dq).transpose(1, 0, 2, 3, 4)

    def one(qblk):
        s = jnp.einsum('bqhd,bkhd->bhqk', qblk, k, preferred_element_type=jnp.float32) * scale
        p = jax.nn.softmax(s, axis=-1).astype(v.dtype)
        return jnp.einsum('bhqk,bkhd->bqhd', p, v)

    o = lax.map(one, qb)
    return o.transpose(1, 0, 2, 3, 4).reshape(b, n, h, v.shape[-1])


def neighbourhood_attention(q, k, v, k_ctx, v_ctx, rpb):
    b, s, h, d = q.shape
    rows = s // GRID_W
    wr = min(NA_WIN_R, rows)
    wc = min(NA_WIN_C, GRID_W)
    n_ctx = k_ctx.shape[1]
    qg = q.reshape(b, rows, GRID_W, h, d).transpose(1, 0, 2, 3, 4)
    kg = k.reshape(b, rows, GRID_W, h, d)
    vg = v.reshape(b, rows, GRID_W, h, d)
    cols = jnp.arange(GRID_W)
    c0 = jnp.clip(cols - wc // 2, 0, GRID_W - wc)
    col_idx = c0[:, None] + jnp.arange(wc)[None, :]
    col_off = col_idx - cols[:, None] + (NA_WIN_C - 1)
    rpb32 = rpb.astype(jnp.float32)

    def one(args):
        r, qr = args
        r0 = jnp.clip(r - wr // 2, 0, rows - wr)
        kw = jnp.take(lax.dynamic_slice_in_dim(kg, r0, wr, axis=1), col_idx, axis=2)
        vw = jnp.take(lax.dynamic_slice_in_dim(vg, r0, wr, axis=1), col_idx, axis=2)
        row_off = r0 + jnp.arange(wr) - r + (NA_WIN_R - 1)
        bias = rpb32[:, row_off[None, :, None], col_off[:, None, :]]
        s_loc = jnp.einsum('bqhd,baqchd->bhqac', qr, kw, preferred_element_type=jnp.float32) * NA_SCALE + bias
        s_ctx = jnp.einsum('bqhd,bkhd->bhqk', qr, k_ctx, preferred_element_type=jnp.float32) * NA_SCALE
        p = jax.nn.softmax(jnp.concatenate([s_ctx, s_loc.reshape(b, h, GRID_W, wr * wc)], axis=-1), axis=-1)
        p = p.astype(v.dtype)
        p_ctx = p[..., :n_ctx]
        p_loc = p[..., n_ctx:].reshape(b, h, GRID_W, wr, wc)
        return (jnp.einsum('bhqk,bkhd->bqhd', p_ctx, v_ctx)
                + jnp.einsum('bhqac,baqchd->bqhd', p_loc, vw))

    o = lax.map(one, (jnp.arange(rows), qg))
    return o.transpose(1, 0, 2, 3, 4).reshape(b, s, h, d)


def short_gated_conv(p_b, p_c, p_x, w):
    n = p_x.shape[1]
    u = jnp.pad(p_c * p_x, ((0, 0), (CONV_W // 2, CONV_W // 2), (0, 0)))
    y = sum(w[i] * u[:, i:i + n] for i in range(CONV_W))
    return p_b * y


def expert_choice_moe(hn, w_router, w_gate, w_up, w_down):
    b, n, _ = hn.shape
    cap = CAP_FACTOR * n // N_EXPERTS
    aff = jax.nn.softmax(jnp.einsum('bnd,de->bne', hn, w_router, preferred_element_type=jnp.float32), axis=-1)
    g, idx = lax.top_k(aff.transpose(0, 2, 1), cap)
    xe = jax.vmap(lambda hb, ib: hb[ib])(hn, idx)
    a = jnp.einsum('becd,edf->becf', xe, w_gate)
    u = jnp.einsum('becd,edf->becf', xe, w_up)
    y = jnp.einsum('becf,efd->becd', jax.nn.silu(a) * u, w_down) * g[..., None].astype(hn.dtype)
    bidx = jnp.arange(b)[:, None, None]
    return jnp.zeros_like(hn).at[bidx, idx].add(y)


def setup_inputs(seed: int = 0) -> dict:
    key = jax.random.key(seed)
    ks = iter(jax.random.split(key, 32))
    L = DEPTH

    def nrm(shape, scale):
        return jax.random.normal(next(ks), shape, jnp.float32) * scale

    def gain(shape):
        return 1.0 + nrm(shape, 0.02)

    return {
        "x": nrm((BATCH, SEQ, D_MODEL), 1.0),
        "c": nrm((BATCH, D_MODEL), 1.0),
        "ctx": nrm((BATCH, CTX_LEN, D_MODEL), 1.0),
        "c_ctx": nrm((D_MODEL,), 1.0),
        "ada_down": nrm((L, D_MODEL, ADA_RANK), D_MODEL ** -0.5),
        "ada_up": nrm((L, ADA_RANK, N_MOD * D_MODEL), 0.3 * ADA_RANK ** -0.5),
        "ada_bias": nrm((L, N_MOD * D_MODEL), 0.01),
        "norm1_g": gain((L, D_MODEL)),
        "w_in": nrm((L, D_MODEL, D_IN), D_MODEL ** -0.5),
        "mla_qa_g": gain((L, MLA_Q_RANK)),
        "mla_kva_g": gain((L, MLA_KV_RANK)),
        "mla_w_uq": nrm((L, MLA_Q_RANK, MLA_HEADS * MLA_QK), MLA_Q_RANK ** -0.5),
        "mla_w_ukv": nrm((L, MLA_KV_RANK, MLA_HEADS * (MLA_NOPE + MLA_V)), MLA_KV_RANK ** -0.5),
        "mla_q_g": gain((L, MLA_QK)),
        "mla_k_g": gain((L, MLA_QK)),
        "conv_w": nrm((L, CONV_W, CONV_CH), CONV_W ** -0.5),
        "na_q_g": gain((L, NA_DIM)),
        "na_k_g": gain((L, NA_DIM)),
        "na_rpb": nrm((L, NA_HEADS, 2 * NA_WIN_R - 1, 2 * NA_WIN_C - 1), 0.2),
        "w_out": nrm((L, D_MODEL, D_MODEL), D_MODEL ** -0.5),
        "norm2_g": gain((L, D_MODEL)),
        "w_router": nrm((L, D_MODEL, N_EXPERTS), D_MODEL ** -0.5),
        "ex_gate": nrm((L, N_EXPERTS, D_MODEL, EXPERT_FF), D_MODEL ** -0.5),
        "ex_up": nrm((L, N_EXPERTS, D_MODEL, EXPERT_FF), D_MODEL ** -0.5),
        "ex_down": nrm((L, N_EXPERTS, EXPERT_FF, D_MODEL), EXPERT_FF ** -0.5),
    }


def reference(x, c, ctx, c_ctx, ada_down, ada_up, ada_bias, norm1_g, w_in, mla_qa_g, mla_kva_g,
              mla_w_uq, mla_w_ukv, mla_q_g, mla_k_g, conv_w, na_q_g, na_k_g, na_rpb, w_out,
              norm2_g, w_router, ex_gate, ex_up, ex_down):
    b, s, _ = x.shape
    rope = axial_rope_tables(s)
    h, hc = x, ctx
    for l in range(DEPTH):
        last = l == DEPTH - 1
        sx1, cx1, gx1, sx2, cx2, gx2 = ada_mod(c[:, None, :], ada_down[l], ada_up[l], ada_bias[l])
        sc1, cc1, gc1, sc2, cc2, gc2 = ada_mod(c_ctx, ada_down[l], ada_up[l], ada_bias[l])
        w = w_in[l]

        nc = modulate(hc, norm1_g[l], sc1, cc1)
        if last:
            pc_mla_kv = nc @ w[:, OFF_CKV:OFF_CB]
            pc_na_kv = nc @ w[:, OFF_NK:D_IN]
        else:
            pc = nc @ w
            pc_mla_kv = pc[..., OFF_CKV:OFF_CB]
            pc_na_kv = pc[..., OFF_NK:]
        kc_a, vc_a = mla_kv(pc_mla_kv, mla_kva_g[l], mla_w_ukv[l], mla_k_g[l], None)
        kc_c, vc_c = na_kv(pc_na_kv, na_k_g[l])

        nx = modulate(h, norm1_g[l], sx1, cx1)
        px = nx @ w
        q_a = mla_q(px[..., OFF_CQ:OFF_CKV], mla_qa_g[l], mla_w_uq[l], mla_q_g[l], rope)
        k_a, v_a = mla_kv(px[..., OFF_CKV:OFF_CB], mla_kva_g[l], mla_w_ukv[l], mla_k_g[l], rope)
        o_a = dense_attention(q_a, jnp.concatenate([kc_a, k_a], axis=1),
                              jnp.concatenate([vc_a, v_a], axis=1), MLA_SCALE)
        o_b = short_gated_conv(px[..., OFF_CB:OFF_CC], px[..., OFF_CC:OFF_CX], px[..., OFF_CX:OFF_NQ], conv_w[l])
        q_c = na_q(px[..., OFF_NQ:OFF_NK], na_q_g[l])
        k_c, v_c = na_kv(px[..., OFF_NK:], na_k_g[l])
        o_c = neighbourhood_attention(q_c, k_c, v_c, kc_c, vc_c, na_rpb[l])
        mix = jnp.concatenate([o_a.reshape(b, s, -1), o_b, o_c.reshape(b, s, -1)], axis=-1)
        h = h + gx1 * (mix @ w_out[l])
        h = h + gx2 * expert_choice_moe(modulate(h, norm2_g[l], sx2, cx2),
                                        w_router[l], ex_gate[l], ex_up[l], ex_down[l])

        if not last:
            n_ctx = hc.shape[1]
            qc_a = mla_q(pc[..., OFF_CQ:OFF_CKV], mla_qa_g[l], mla_w_uq[l], mla_q_g[l], None)
            oc_a = dense_attention(qc_a, kc_a, vc_a, MLA_SCALE)
            oc_b = short_gated_conv(pc[..., OFF_CB:OFF_CC], pc[..., OFF_CC:OFF_CX], pc[..., OFF_CX:OFF_NQ], conv_w[l])
            qc_c = na_q(pc[..., OFF_NQ:OFF_NK], na_q_g[l])
            oc_c = dense_attention(qc_c, kc_c, vc_c, NA_SCALE)
            mix_c = jnp.concatenate([oc_a.reshape(b, n_ctx, -1), oc_b, oc_c.reshape(b, n_ctx, -1)], axis=-1)
            hc = hc + gc1 * (mix_c @ w_out[l])
            hc = hc + gc2 * expert_choice_moe(modulate(hc, norm2_g[l], sc2, cc2),
                                              w_router[l], ex_gate[l], ex_up[l], ex_down[l])
    return h
```

```python
import numpy as np
import ml_dtypes
from contextlib import ExitStack
import concourse.bass as bass
import concourse.mybir as mybir
from concourse.bass_utils import run_bass_kernel_spmd

F32 = mybir.dt.float32
BF16 = mybir.dt.bfloat16
I32 = mybir.dt.int32
ALU = mybir.AluOpType
AF = mybir.ActivationFunctionType
AX = mybir.AxisListType

ENG_ATTR = {'pe': 'tensor', 'act': 'scalar', 'dve': 'vector', 'pool': 'gpsimd', 'sp': 'sync'}
COMPUTE = ('pe', 'act', 'dve', 'pool')
NDS = 12


class Buf:
    __slots__ = ('ap', 'w', 'r', 'name')

    def __init__(self, ap, name=''):
        self.ap = ap
        self.w = None
        self.r = []
        self.name = name

    def __getitem__(self, k):
        return self.ap[k]


class Prog:
    def __init__(self, nc):
        self.nc = nc
        self.ops = {e: [] for e in ENG_ATTR}
        self.cnt = {e: 0 for e in COMPUTE}
        self.seen = {e: {} for e in ENG_ATTR}
        self.dval = {}
        self.drr = {e: 0 for e in ENG_ATTR}
        self.sems = {}
        self.top = ExitStack()
        for e in COMPUTE:
            self.sems[('c', e)] = self.top.enter_context(nc.semaphore('c_' + e))
        for e in ('sp', 'pool', 'act'):
            for i in range(NDS):
                k = ('d', e, i)
                self.sems[k] = self.top.enter_context(nc.semaphore('d_%s_%d' % (e, i)))
                self.dval[k] = 0
        self.nops = 0
        self.regs = {}

    def close(self):
        self.top.close()

    def _need(self, eng, k, v, waits):
        if v <= 0:
            return
        if self.seen[eng].get(k, 0) >= v:
            return
        self.seen[eng][k] = v
        waits.append((k, v))

    def i(self, eng, meth, reads=(), writes=(), **kw):
        dma = meth in ('dma_start', 'indirect_dma_start', 'dma_start_transpose')
        return self.op(eng, (meth, kw), reads, writes, dma)

    def op(self, eng, fn, reads=(), writes=(), dma=False):
        deps = []
        for b in reads:
            if b.w is not None:
                deps.append(b.w)
        for b in writes:
            if b.w is not None:
                deps.append(b.w)
            deps.extend(b.r)
        waits = []
        dmax = {}
        for (k, v) in deps:
            if k == ('c', 'pe') and eng == 'pe' and not dma:
                continue
            if dmax.get(k, 0) < v:
                dmax[k] = v
        for k, v in dmax.items():
            self._need(eng, k, v, waits)
        if dma:
            i = self.drr[eng]
            self.drr[eng] = (i + 1) % NDS
            k = ('d', eng, i)
            self._need(eng, k, self.dval[k], waits)
            self.dval[k] += 16
            tok = (k, self.dval[k])
        else:
            self.cnt[eng] += 1
            tok = (('c', eng), self.cnt[eng])
        self.ops[eng].append((waits, fn, tok))
        self.nops += 1
        for b in reads:
            b.r.append(tok)
        for b in writes:
            b.w = tok
            b.r = []
        return tok

    def barrier(self):
        for e in ENG_ATTR:
            waits = []
            for x in COMPUTE:
                if x != e:
                    self._need(e, ('c', x), self.cnt[x], waits)
            for k, v in self.dval.items():
                self._need(e, k, v, waits)
            if waits:
                self.ops[e].append((waits, None, None))

    def emit(self):
        self.barrier()
        nc = self.nc
        with nc.Block() as block:
            for e, attr in ENG_ATTR.items():
                ops = self.ops[e]
                if not ops:
                    continue

                def run(engobj, ops=ops):
                    for waits, fn, tok in ops:
                        for (k, v) in waits:
                            engobj.wait_ge(self.sems[k], v)
                        if fn is not None:
                            if isinstance(fn, tuple):
                                try:
                                    kw = fn[1]
                                    if isinstance(kw.get('bounds_check'), int):
                                        rk = ('reg', kw['bounds_check'])
                                        if rk not in self.regs:
                                            self.regs[rk] = engobj.to_reg(kw['bounds_check'])
                                        kw = dict(kw)
                                        kw['bounds_check'] = self.regs[rk]
                                    ins = getattr(engobj, fn[0])(**kw)
                                except Exception:
                                    print("FAILED OP", fn[0])
                                    raise
                            else:
                                ins = fn(engobj)
                            ins.then_inc(self.sems[tok[0]], 16 if tok[0][0] == 'd' else 1)
                getattr(block, attr)(run)
        self.ops = {e: [] for e in ENG_ATTR}


_UID = [0]


class Scope:
    def __init__(self, P):
        self.P = P
        self.st = ExitStack()
        self.n = 0

    def __enter__(self):
        self.st.__enter__()
        return self

    def __exit__(self, *a):
        if a[0] is None:
            self.P.emit()
        return self.st.__exit__(*a)

    def _nm(self, p):
        _UID[0] += 1
        return '%s_%d' % (p, _UID[0])

    def sb(self, shape, dt, name=None):
        self.n += 1
        t = self.st.enter_context(self.P.nc.sbuf_tensor(name or self._nm('sb'), list(shape), dt))
        return t

    def ps(self, shape, dt, name=None):
        self.n += 1
        t = self.st.enter_context(self.P.nc.psum_tensor(name or self._nm('ps'), list(shape), dt))
        return t

    def sbuf(self, shape, dt, name=None):
        return Buf(self.sb(shape, dt, name))

    def psum(self, shape, dt, name=None):
        return Buf(self.ps(shape, dt, name))


D = 4096
DIN = 9280
NTOK = 16896
NTILE = 132
EPS = 1e-6
OFF = dict(cq=0, ckv=1024, kr=1536, cb=1600, cc=2624, cx=3648, nq=4672, nk=6208, nv=7744)


def din(nc, name, shape, dt=F32):
    return nc.dram_tensor(name, list(shape), dt, kind="ExternalInput")


def dout(nc, name, shape, dt=F32):
    return nc.dram_tensor(name, list(shape), dt, kind="ExternalOutput")


class RR:
    def __init__(self, items):
        self.items = items
        self.k = 0

    def next(self):
        x = self.items[self.k % len(self.items)]
        self.k += 1
        return x


def tile_cond(ti):
    b, j = divmod(ti, 66)
    return 2 if j >= 64 else b


def rstd_from_ssq(P, ssq, dim):
    P.i('dve', 'tensor_scalar', reads=[ssq], writes=[ssq], out=ssq[:], in0=ssq[:], scalar1=1.0 / dim, scalar2=EPS, op0=ALU.mult, op1=ALU.add)
    P.i('act', 'activation', reads=[ssq], writes=[ssq], out=ssq[:], in_=ssq[:], func=AF.Sqrt)
    P.i('dve', 'reciprocal', reads=[ssq], writes=[ssq], out=ssq[:], in_=ssq[:])


def transposes(P, tps, evs, blocks, ident):
    for g0 in range(0, len(blocks), 4):
        grp = blocks[g0:g0 + 4]
        tp = tps.next()
        for q, (sb_, sap, db_, dap) in enumerate(grp):
            w = sap.shape[-1]
            P.i('pe', 'transpose', reads=[sb_, ident], writes=[tp], out=tp[0:w, q * 128:(q + 1) * 128], in_=sap, identity=ident[:])
        for q, (sb_, sap, db_, dap) in enumerate(grp):
            w = sap.shape[-1]
            ev = evs.next()
            if ev == 'act':
                P.i('act', 'copy', reads=[tp], writes=[db_], out=dap, in_=tp[0:w, q * 128:(q + 1) * 128])
            else:
                P.i('dve', 'tensor_copy', reads=[tp], writes=[db_], out=dap, in_=tp[0:w, q * 128:(q + 1) * 128])


def phase_ada(P, nc, T):
    with Scope(P) as S:
        sc = S.sbuf([128, 32, 3], F32)
        sg = S.sbuf([128, 32, 3], F32)
        dn = RR([S.sbuf([128, 8, 256], F32) for _ in range(2)])
        upb = RR([S.sbuf([128, 2, 2048], F32) for _ in range(2)])
        tT = S.sbuf([128, 2, 3], F32)
        tmp = RR([S.sbuf([3, 512], F32) for _ in range(2)])
        bbs = RR([S.sbuf([3, 2048], F32) for _ in range(2)])
        gg = S.sbuf([3, 2, D], F32)
        res = S.sbuf([3, 6, D], F32)
        pt = [S.psum([128, 4], F32) for _ in range(2)]
        pm = RR([S.psum([3, 512], F32) for _ in range(2)])
        P.i('sp', 'dma_start', writes=[sc], out=sc[:], in_=T['cT'].ap())
        P.i('act', 'activation', reads=[sc], writes=[sg], out=sg[:], in_=sc[:], func=AF.Sigmoid)
        P.i('dve', 'tensor_tensor', reads=[sg, sc], writes=[sc], out=sc[:], in0=sc[:], in1=sg[:], op=ALU.mult)
        P.i('sp', 'dma_start', writes=[gg], out=gg[:, 0, :], in_=T['g1_3'].ap())
        P.i('sp', 'dma_start', writes=[gg], out=gg[:, 1, :], in_=T['g2_3'].ap())
        dview = T['ada_down'].ap().rearrange("(kc p) r -> p kc r", p=128)
        for rc in range(2):
            pp = pt[rc]
            for k4 in range(4):
                db = dn.next()
                P.i('sp', 'dma_start', writes=[db], out=db[:], in_=dview[:, k4 * 8:(k4 + 1) * 8, :])
                for k in range(8):
                    kc = k4 * 8 + k
                    P.i('pe', 'matmul', reads=[db, sc], writes=[pp], out=pp[:, 0:3], lhsT=db[:, k, rc * 128:(rc + 1) * 128],
                        rhs=sc[:, kc, :], start=(kc == 0), stop=(kc == 31))
            P.i('dve', 'tensor_copy', reads=[pp], writes=[tT], out=tT[:, rc, :], in_=pp[:, 0:3])
        uview = T['ada_up'].ap().rearrange("(rc p) n -> p rc n", p=128)
        for nb in range(12):
            ub = upb.next()
            P.i('sp', 'dma_start', writes=[ub], out=ub[:], in_=uview[:, :, nb * 2048:(nb + 1) * 2048])
            bb = bbs.next()
            P.i('sp', 'dma_start', writes=[bb], out=bb[:], in_=T['bias3'].ap()[:, nb * 2048:(nb + 1) * 2048])
            for q in range(4):
                pq = pm.next()
                for rc in range(2):
                    P.i('pe', 'matmul', reads=[tT, ub], writes=[pq], out=pq[:, :], lhsT=tT[:, rc, :], rhs=ub[:, rc, q * 512:(q + 1) * 512],
                        start=(rc == 0), stop=(rc == 1))
                n0 = nb * 2048 + q * 512
                tb = tmp.next()
                P.i('dve', 'tensor_tensor', reads=[pq, bb], writes=[tb], out=tb[:, :], in0=pq[:, :], in1=bb[:, q * 512:(q + 1) * 512], op=ALU.add)
                c = n0 // D
                o0 = n0 % D
                dst = [1, 0, 2, 4, 3, 5][c]
                if c in (1, 4):
                    gi = 0 if c == 1 else 1
                    P.i('dve', 'scalar_tensor_tensor', reads=[tb, gg], writes=[res], out=res[:, dst, o0:o0 + 512], in0=tb[:, :], scalar=1.0,
                        in1=gg[:, gi, o0:o0 + 512], op0=ALU.add, op1=ALU.mult)
                else:
                    P.i('dve', 'tensor_copy', reads=[tb], writes=[res], out=res[:, dst, o0:o0 + 512], in_=tb[:, :])
        P.i('sp', 'dma_start', reads=[res], out=T['modv'].ap(), in_=res[:])


def load_bvec(P, T, dst, cond, s, eng='sp'):
    src = T['modv'].ap()[cond, s:s + 1, :].partition_broadcast(128)
    P.i(eng, 'dma_start', writes=[dst], out=dst[:], in_=src)


def norm_mod_tile(P, ht, xh, ssq, mulb, shiftb):
    P.i('dve', 'scalar_tensor_tensor', reads=[ht], writes=[xh, ssq], out=xh[:], in0=ht[:], scalar=1.0, in1=ht[:], op0=ALU.mult, op1=ALU.mult,
        accum_out=ssq[:, 0:1])
    rstd_from_ssq(P, ssq, D)
    P.i('act', 'activation', reads=[ht, ssq], writes=[xh], out=xh[:], in_=ht[:], func=AF.Copy, scale=ssq[:, 0:1])
    P.i('dve', 'tensor_tensor', reads=[xh, mulb], writes=[xh], out=xh[:], in0=xh[:], in1=mulb[:], op=ALU.mult)
    P.i('pool', 'tensor_tensor', reads=[xh, shiftb], writes=[xh], out=xh[:], in0=xh[:], in1=shiftb[:], op=ALU.add)


def phase_win(P, nc, T, tiles_per_group=12, tiles=None):
    NCH = 20
    CW = 464
    wv = T['w_in'].ap().rearrange("(kc p) n -> p kc n", p=128)
    tiles = list(range(NTILE)) if tiles is None else tiles
    groups = [tiles[g:g + tiles_per_group] for g in range(0, len(tiles), tiles_per_group)]
    cur = [None]
    for grp in groups:
        with Scope(P) as G:
            nxT = G.sbuf([128, 32, tiles_per_group * 128], BF16)
            with Scope(P) as S:
                mulb = S.sbuf([128, D], F32)
                shiftb = S.sbuf([128, D], F32)
                hts = RR([S.sbuf([128, D], F32) for _ in range(2)])
                xh = S.sbuf([128, D], F32)
                ssqs = RR([S.sbuf([128, 1], F32) for _ in range(2)])
                identf = S.sbuf([128, 128], F32)
                tps = RR([S.psum([128, 512], F32) for _ in range(2)])
                evs = RR(['act', 'dve'])
                P.i('sp', 'dma_start', writes=[identf], out=identf[:], in_=T['identf'].ap())
                cond_loaded = None
                for li, ti in enumerate(grp):
                    cond = tile_cond(ti)
                    if cond != cond_loaded:
                        load_bvec(P, T, mulb, cond, 0)
                        load_bvec(P, T, shiftb, cond, 1)
                        cond_loaded = cond
                    ht = hts.next()
                    ssq = ssqs.next()
                    P.i('sp', 'dma_start', writes=[ht], out=ht[:], in_=T['h'].ap()[ti * 128:(ti + 1) * 128, :])
                    norm_mod_tile(P, ht, xh, ssq, mulb, shiftb)
                    blocks = [(xh, xh[:, kc * 128:(kc + 1) * 128], nxT, nxT[:, kc, li * 128:(li + 1) * 128]) for kc in range(32)]
                    transposes(P, tps, evs, blocks, identf)
            with Scope(P) as S:
                wbs = RR([S.sbuf([128, 32, CW], BF16) for _ in range(2)])
                accs = RR([S.psum([128, CW], F32) for _ in range(4)])
                osts = RR([S.sbuf([128, CW], F32) for _ in range(4)])
                evs = RR(['act', 'dve'])
                for ch in range(NCH):
                    n0 = ch * CW
                    wb = wbs.next()
                    for s4 in range(4):
                        P.i('pool', 'dma_start', writes=[wb], out=wb[:, s4 * 8:(s4 + 1) * 8, :], in_=wv[:, s4 * 8:(s4 + 1) * 8, n0:n0 + CW])
                    for li, ti in enumerate(grp):
                        acc = accs.next()
                        for kc in range(32):
                            P.i('pe', 'matmul', reads=[nxT, wb], writes=[acc], out=acc[:], lhsT=nxT[:, kc, li * 128:(li + 1) * 128], rhs=wb[:, kc, :],
                                start=(kc == 0), stop=(kc == 31))
                        ost = osts.next()
                        if evs.next() == 'act':
                            P.i('act', 'copy', reads=[acc], writes=[ost], out=ost[:], in_=acc[:])
                        else:
                            P.i('dve', 'tensor_copy', reads=[acc], writes=[ost], out=ost[:], in_=acc[:])
                        P.i('sp', 'dma_start', reads=[ost], out=T['px'][ch].ap()[ti * 128:(ti + 1) * 128, :], in_=ost[:])


def make_T(nc):
    T = {}
    T['h'] = din(nc, 'h', [NTOK, D])
    T['cT'] = din(nc, 'cT', [128, 32, 3])
    T['ada_down'] = din(nc, 'ada_down', [D, 256])
    T['ada_up'] = din(nc, 'ada_up', [256, 6 * D])
    T['bias3'] = din(nc, 'bias3', [3, 6 * D])
    T['g1_3'] = din(nc, 'g1_3', [3, D])
    T['g2_3'] = din(nc, 'g2_3', [3, D])
    T['w_in'] = din(nc, 'w_in', [D, DIN])
    T['identf'] = din(nc, 'identf', [128, 128])
    T['modv'] = nc.dram_tensor('modv', [3, 6, D], F32)
    T['modv_b'] = Buf(None)
    T['px'] = [nc.dram_tensor('px%d' % i, [NTOK, 464], F32) for i in range(20)]
    return T


def load_px_cols(P, T, dst, dst_ap_fn, r0, c0, c1, eng='sp'):
    c = c0
    while c < c1:
        ch = c // 464
        e = min(c1, (ch + 1) * 464)
        P.i(eng, 'dma_start', writes=[dst], out=dst_ap_fn(c - c0, e - c0), in_=T['px'][ch].ap()[r0:r0 + 128, c - ch * 464:e - ch * 464])
        c = e


def make_T2(nc, T):
    for name, shape in (('w_uq', [1024, 2304]), ('w_ukv', [512, 3072]), ('gqa', [128, 1024]), ('gkva', [128, 512]),
                        ('gq12', [128, 2304]), ('gk12', [128, 2304]), ('gnq', [128, 1536]), ('gnk', [128, 1536]), ('cs', [NTOK, 64])):
        T[name] = din(nc, name, shape)
    T['identb'] = din(nc, 'identb', [128, 128], BF16)
    T['qaT'] = nc.dram_tensor('qaT', [12, 192, NTOK], BF16)
    T['kaT'] = nc.dram_tensor('kaT', [12, 192, NTOK], BF16)
    T['va'] = nc.dram_tensor('va', [NTOK, 1536], BF16)
    T['qcT'] = nc.dram_tensor('qcT', [12, 128, NTOK], BF16)
    T['kcT'] = nc.dram_tensor('kcT', [12, 128, NTOK], BF16)
    T['vc'] = nc.dram_tensor('vc', [NTOK, 1536], BF16)
    T['uT'] = nc.dram_tensor('uT', [1024, NTOK], F32)
    T['pbT'] = nc.dram_tensor('pbT', [1024, NTOK], F32)


def head_norm(P, x, x3, sq, sh, hd, gain):
    P.i('act', 'activation', reads=[x], writes=[sq], out=sq[:, 0:12 * hd], in_=x[:], func=AF.Square)
    P.i('dve', 'tensor_reduce', reads=[sq], writes=[sh], out=sh[:], in_=sq[:, 0:12 * hd].rearrange("p (h d) -> p h d", h=12), axis=AX.X, op=ALU.add)
    rstd_from_ssq(P, sh, hd)
    P.i('dve', 'tensor_tensor', reads=[x, sh], writes=[x], out=x3, in0=x3, in1=sh[:].unsqueeze(2).broadcast_to([128, 12, hd]), op=ALU.mult)
    P.i('pool', 'tensor_tensor', reads=[x, gain], writes=[x], out=x[:], in0=x[:], in1=gain[:], op=ALU.mult)


def rope_tail(P, src, src3, dst, dst3, cst, tmps):
    t1, t2, t3, t4 = tmps
    for a in range(2):
        o = 128 + 32 * a
        x1 = src3[:, :, o:o + 16]
        x2 = src3[:, :, o + 16:o + 32]
        cos = cst[:, 32 * a:32 * a + 16].unsqueeze(1).broadcast_to([128, 12, 16])
        sin = cst[:, 32 * a + 16:32 * a + 32].unsqueeze(1).broadcast_to([128, 12, 16])
        P.i('dve', 'tensor_tensor', reads=[src, cst], writes=[t1], out=t1[:], in0=x1, in1=cos, op=ALU.mult)
        P.i('pool', 'tensor_tensor', reads=[src, cst], writes=[t2], out=t2[:], in0=x2, in1=sin, op=ALU.mult)
        P.i('dve', 'tensor_tensor', reads=[t1, t2], writes=[dst], out=dst3[:, :, o:o + 16], in0=t1[:], in1=t2[:], op=ALU.subtract)
        P.i('dve', 'tensor_tensor', reads=[src, cst], writes=[t3], out=t3[:], in0=x2, in1=cos, op=ALU.mult)
        P.i('pool', 'tensor_tensor', reads=[src, cst], writes=[t4], out=t4[:], in0=x1, in1=sin, op=ALU.mult)
        P.i('dve', 'tensor_tensor', reads=[t3, t4], writes=[dst], out=dst3[:, :, o + 16:o + 32], in0=t3[:], in1=t4[:], op=ALU.add)


def phase_prep(P, nc, T, tiles=None):
    tiles = list(range(NTILE)) if tiles is None else tiles
    with Scope(P) as S:
        wuq = S.sbuf([128, 8, 2304], BF16)
        wukv = S.sbuf([128, 4, 3072], BF16)
        gqa = S.sbuf([128, 1024], F32)
        gkva = S.sbuf([128, 512], F32)
        gq12 = S.sbuf([128, 2304], F32)
        gk12 = S.sbuf([128, 2304], F32)
        gnq = S.sbuf([128, 1536], F32)
        gnk = S.sbuf([128, 1536], F32)
        identf = S.sbuf([128, 128], F32)
        identb = S.sbuf([128, 128], BF16)
        wq_v = T['w_uq'].ap().rearrange("(kc p) n -> p kc n", p=128)
        for k2 in range(4):
            P.i('pool', 'dma_start', writes=[wuq], out=wuq[:, 2 * k2:2 * k2 + 2, :], in_=wq_v[:, 2 * k2:2 * k2 + 2, :])
        wk_v = T['w_ukv'].ap().rearrange("(kc p) n -> p kc n", p=128)
        for k2 in range(2):
            P.i('pool', 'dma_start', writes=[wukv], out=wukv[:, 2 * k2:2 * k2 + 2, :], in_=wk_v[:, 2 * k2:2 * k2 + 2, :])
        for sbt, nm in ((gqa, 'gqa'), (gkva, 'gkva'), (gq12, 'gq12'), (gk12, 'gk12'), (gnq, 'gnq'), (gnk, 'gnk'), (identf, 'identf'), (identb, 'identb')):
            P.i('sp', 'dma_start', writes=[sbt], out=sbt[:], in_=T[nm].ap())
        pas = RR([S.sbuf([128, 1600], F32) for _ in range(2)])
        csts = RR([S.sbuf([128, 64], F32) for _ in range(2)])
        pns = RR([S.sbuf([128, 1536], F32) for _ in range(2)])
        cqn = S.sbuf([128, 1024], F32)
        cT_ = S.sbuf([128, 8, 128], BF16)
        raw = S.sbuf([128, 3072], F32)
        sq = S.sbuf([128, 3072], F32)
        sh = S.sbuf([128, 12], F32)
        s1 = S.sbuf([128, 1], F32)
        s2 = S.sbuf([128, 1], F32)
        kt = S.sbuf([128, 2304], F32)
        qo = S.sbuf([128, 2304], BF16)
        vo = S.sbuf([128, 1536], BF16)
        no = S.sbuf([128, 1536], BF16)
        qTn = S.sbuf([128, 12, 128], BF16)
        qTr = S.sbuf([64, 12, 128], BF16)
        tmps = [S.sbuf([128, 12, 16], F32) for _ in range(4)]
        cvT = S.sbuf([128, 8, 128], F32)
        tpf = RR([S.psum([128, 512], F32) for _ in range(2)])
        tpb = RR([S.psum([128, 512], BF16) for _ in range(2)])
        accs = RR([S.psum([128, 512], F32) for _ in range(3)])
        evs = RR(['act', 'dve'])
        qo3 = qo[:].rearrange("p (h d) -> p h d", h=12)

        def to_featmajor(dst_dram, r0, hd_main, tail):
            blocks = []
            for h in range(12):
                blocks.append((qo, qo3[:, h, 0:128] if tail else qo[:, h * 128:(h + 1) * 128], qTn, qTn[:, h, :]))
            if tail:
                for h in range(12):
                    blocks.append((qo, qo3[:, h, 128:192], qTr, qTr[0:64, h, :]))
            transposes(P, tpb, evs, blocks, identb)
            P.i('sp', 'dma_start', reads=[qTn], out=dst_dram.ap()[:, 0:128, r0:r0 + 128].rearrange("h d t -> d h t"), in_=qTn[:])
            if tail:
                P.i('sp', 'dma_start', reads=[qTr], out=dst_dram.ap()[:, 128:192, r0:r0 + 128].rearrange("h d t -> d h t"), in_=qTr[:])

        for ti in tiles:
            r0 = ti * 128
            pa = pas.next()
            cst = csts.next()
            load_px_cols(P, T, pa, lambda a, b, pa=pa: pa[:, a:b], r0, 0, 1600)
            P.i('sp', 'dma_start', writes=[cst], out=cst[:], in_=T['cs'].ap()[r0:r0 + 128, :])
            P.i('dve', 'scalar_tensor_tensor', reads=[pa], writes=[cqn, s1], out=cqn[:], in0=pa[:, 0:1024], scalar=1.0, in1=pa[:, 0:1024],
                op0=ALU.mult, op1=ALU.mult, accum_out=s1[:, 0:1])
            rstd_from_ssq(P, s1, 1024)
            P.i('dve', 'scalar_tensor_tensor', reads=[pa, s1, gqa], writes=[cqn], out=cqn[:], in0=pa[:, 0:1024], scalar=s1[:, 0:1], in1=gqa[:],
                op0=ALU.mult, op1=ALU.mult)
            transposes(P, tpf, evs, [(cqn, cqn[:, kc * 128:(kc + 1) * 128], cT_, cT_[:, kc, :]) for kc in range(8)], identf)
            for g in range(6):
                acc = accs.next()
                for kc in range(8):
                    P.i('pe', 'matmul', reads=[cT_, wuq], writes=[acc], out=acc[:, 0:384], lhsT=cT_[:, kc, :], rhs=wuq[:, kc, g * 384:(g + 1) * 384],
                        start=(kc == 0), stop=(kc == 7))
                P.i('act', 'copy', reads=[acc], writes=[raw], out=raw[:, g * 384:(g + 1) * 384], in_=acc[:, 0:384])
            q3 = raw[:, 0:2304].rearrange("p (h d) -> p h d", h=12)
            P.i('act', 'activation', reads=[raw], writes=[sq], out=sq[:, 0:2304], in_=raw[:, 0:2304], func=AF.Square)
            P.i('dve', 'tensor_reduce', reads=[sq], writes=[sh], out=sh[:], in_=sq[:, 0:2304].rearrange("p (h d) -> p h d", h=12), axis=AX.X, op=ALU.add)
            rstd_from_ssq(P, sh, 192)
            P.i('dve', 'tensor_tensor', reads=[raw, sh], writes=[raw], out=q3, in0=q3, in1=sh[:].unsqueeze(2).broadcast_to([128, 12, 192]), op=ALU.mult)
            P.i('pool', 'tensor_tensor', reads=[raw, gq12], writes=[raw], out=raw[:, 0:2304], in0=raw[:, 0:2304], in1=gq12[:], op=ALU.mult)
            P.i('act', 'copy', reads=[raw], writes=[qo], out=qo3[:, :, 0:128], in_=q3[:, :, 0:128])
            rope_tail(P, raw, q3, qo, qo3, cst, tmps)
            to_featmajor(T['qaT'], r0, 128, True)
            P.i('dve', 'scalar_tensor_tensor', reads=[pa], writes=[cqn, s1], out=cqn[:, 0:512], in0=pa[:, 1024:1536], scalar=1.0, in1=pa[:, 1024:1536],
                op0=ALU.mult, op1=ALU.mult, accum_out=s1[:, 0:1])
            rstd_from_ssq(P, s1, 512)
            P.i('dve', 'scalar_tensor_tensor', reads=[pa, s1, gkva], writes=[cqn], out=cqn[:, 0:512], in0=pa[:, 1024:1536], scalar=s1[:, 0:1], in1=gkva[:],
                op0=ALU.mult, op1=ALU.mult)
            transposes(P, tpf, evs, [(cqn, cqn[:, kc * 128:(kc + 1) * 128], cT_, cT_[:, kc, :]) for kc in range(4)], identf)
            for g in range(6):
                acc = accs.next()
                for kc in range(4):
                    P.i('pe', 'matmul', reads=[cT_, wukv], writes=[acc], out=acc[:, :], lhsT=cT_[:, kc, :], rhs=wukv[:, kc, g * 512:(g + 1) * 512],
                        start=(kc == 0), stop=(kc == 3))
                P.i('act', 'copy', reads=[acc], writes=[raw], out=raw[:, g * 512:(g + 1) * 512], in_=acc[:, :])
            kv3 = raw[:].rearrange("p (h d) -> p h d", h=12)
            sq3 = sq[:].rearrange("p (h d) -> p h d", h=12)
            kt3 = kt[:].rearrange("p (h d) -> p h d", h=12)
            P.i('pool', 'tensor_copy', reads=[raw], writes=[vo], out=vo[:].rearrange("p (h d) -> p h d", h=12), in_=kv3[:, :, 128:256])
            P.i('sp', 'dma_start', reads=[vo], out=T['va'].ap()[r0:r0 + 128, :], in_=vo[:])
            P.i('act', 'activation', reads=[raw], writes=[sq], out=sq3[:, :, 0:128], in_=kv3[:, :, 0:128], func=AF.Square)
            P.i('dve', 'tensor_reduce', reads=[sq], writes=[sh], out=sh[:], in_=sq3[:, :, 0:128], axis=AX.X, op=ALU.add)
            P.i('dve', 'scalar_tensor_tensor', reads=[pa], writes=[sq, s2], out=sq[:, 0:64], in0=pa[:, 1536:1600], scalar=1.0, in1=pa[:, 1536:1600],
                op0=ALU.mult, op1=ALU.mult, accum_out=s2[:, 0:1])
            P.i('dve', 'tensor_scalar', reads=[sh, s2], writes=[sh], out=sh[:], in0=sh[:], scalar1=s2[:, 0:1], scalar2=None, op0=ALU.add)
            rstd_from_ssq(P, sh, 192)
            shb = sh[:].unsqueeze(2)
            P.i('dve', 'tensor_tensor', reads=[raw, sh], writes=[kt], out=kt3[:, :, 0:128], in0=kv3[:, :, 0:128], in1=shb.broadcast_to([128, 12, 128]), op=ALU.mult)
            P.i('dve', 'tensor_tensor', reads=[pa, sh], writes=[kt], out=kt3[:, :, 128:192], in0=pa[:, 1536:1600].unsqueeze(1).broadcast_to([128, 12, 64]),
                in1=shb.broadcast_to([128, 12, 64]), op=ALU.mult)
            P.i('pool', 'tensor_tensor', reads=[kt, gk12], writes=[kt], out=kt[:], in0=kt[:], in1=gk12[:], op=ALU.mult)
            P.i('act', 'copy', reads=[kt], writes=[qo], out=qo3[:, :, 0:128], in_=kt3[:, :, 0:128])
            rope_tail(P, kt, kt3, qo, qo3, cst, tmps)
            to_featmajor(T['kaT'], r0, 128, True)
            for which, c0, gain, dstT in (('q', OFF['nq'], gnq, T['qcT']), ('k', OFF['nk'], gnk, T['kcT'])):
                pn = pns.next()
                load_px_cols(P, T, pn, lambda a, b, pn=pn: pn[:, a:b], r0, c0, c0 + 1536)
                pn3 = pn[:].rearrange("p (h d) -> p h d", h=12)
                head_norm(P, pn, pn3, sq, sh, 128, gain)
                P.i('act', 'copy', reads=[pn], writes=[qo], out=qo[:, 0:1536], in_=pn[:])
                to_featmajor(dstT, r0, 128, False)
            pn = pns.next()
            load_px_cols(P, T, pn, lambda a, b, pn=pn: pn[:, a:b], r0, OFF['nv'], OFF['nv'] + 1536)
            P.i('pool', 'tensor_copy', reads=[pn], writes=[no], out=no[:], in_=pn[:])
            P.i('sp', 'dma_start', reads=[no], out=T['vc'].ap()[r0:r0 + 128, :], in_=no[:])
            pb_ = pns.next()
            load_px_cols(P, T, pb_, lambda a, b, pb_=pb_: pb_[:, a:b], r0, OFF['cb'], OFF['cb'] + 1024)
            transposes(P, tpf, evs, [(pb_, pb_[:, kc * 128:(kc + 1) * 128], cvT, cvT[:, kc, :]) for kc in range(8)], identf)
            P.i('sp', 'dma_start', reads=[cvT], out=T['pbT'].ap()[:, r0:r0 + 128].rearrange("(c p) t -> p c t", p=128), in_=cvT[:])
            pc_ = pns.next()
            load_px_cols(P, T, pc_, lambda a, b, pc_=pc_: pc_[:, a:b], r0, OFF['cc'], OFF['cc'] + 1024)
            load_px_cols(P, T, sq, lambda a, b: sq[:, a:b], r0, OFF['cx'], OFF['cx'] + 1024)
            P.i('dve', 'tensor_tensor', reads=[pc_, sq], writes=[pc_], out=pc_[:, 0:1024], in0=pc_[:, 0:1024], in1=sq[:, 0:1024], op=ALU.mult)
            transposes(P, tpf, evs, [(pc_, pc_[:, kc * 128:(kc + 1) * 128], cvT, cvT[:, kc, :]) for kc in range(8)], identf)
            P.i('sp', 'dma_start', reads=[cvT], out=T['uT'].ap()[:, r0:r0 + 128].rearrange("(c p) t -> p c t", p=128), in_=cvT[:])


MLA_SCALE = 192 ** -0.5
NA_SCALE = 128 ** -0.5


def phase_mla(P, nc, T, batches=(0, 1), heads=range(12), qblocks=None):
    with Scope(P) as S:
        kTn = RR([S.sbuf([128, 8448], BF16) for _ in range(2)])
        kTr = RR([S.sbuf([64, 8448], BF16) for _ in range(2)])
        Vs = RR([S.sbuf([128, 66, 128], BF16) for _ in range(2)])
        qn = RR([S.sbuf([128, 512], BF16) for _ in range(2)])
        qr = RR([S.sbuf([64, 512], BF16) for _ in range(2)])
        pTs = RR([S.sbuf([128, 512], BF16) for _ in range(3)])
        ones = S.sbuf([128, 128], BF16)
        rden = S.sbuf([128, 512], F32)
        oo = RR([S.sbuf([128, 512], BF16) for _ in range(2)])
        sts = RR([S.psum([128, 512], F32) for _ in range(2)])
        oaccs = RR([S.psum([128, 512], F32) for _ in range(2)])
        dens = RR([S.psum([128, 512], F32) for _ in range(2)])
        P.i('pool', 'memset', writes=[ones], ap=ones[:], constant=1.0)
        for b in batches:
            base = b * 8448
            for h in heads:
                kn, kr, V = kTn.next(), kTr.next(), Vs.next()
                P.i('sp', 'dma_start', writes=[kn], out=kn[:], in_=T['kaT'].ap()[h, 0:128, base:base + 8448])
                P.i('sp', 'dma_start', writes=[kr], out=kr[:], in_=T['kaT'].ap()[h, 128:192, base:base + 8448])
                for j4 in range(0, 66, 11):
                    P.i('sp', 'dma_start', writes=[V], out=V[:, j4:j4 + 11, :],
                        in_=T['va'].ap()[base + j4 * 128:base + (j4 + 11) * 128, h * 128:(h + 1) * 128].rearrange("(j p) d -> p j d", p=128))
                blocks = [(q0 * 512, 512, list(range(66))) for q0 in range(16)] + [(8192, 256, [64, 65])]
                if qblocks is not None:
                    blocks = [blocks[i] for i in qblocks]
                for (t0, nq, ktiles) in blocks:
                    qa, qb = qn.next(), qr.next()
                    P.i('sp', 'dma_start', writes=[qa], out=qa[:, 0:nq], in_=T['qaT'].ap()[h, 0:128, base + t0:base + t0 + nq])
                    P.i('sp', 'dma_start', writes=[qb], out=qb[:, 0:nq], in_=T['qaT'].ap()[h, 128:192, base + t0:base + t0 + nq])
                    oacc, den = oaccs.next(), dens.next()
                    for ii, kt in enumerate(ktiles):
                        st = sts.next()
                        pT = pTs.next()
                        P.i('pe', 'matmul', reads=[kn, qa], writes=[st], out=st[:, 0:nq], lhsT=kn[:, kt * 128:(kt + 1) * 128], rhs=qa[:, 0:nq], start=True, stop=False)
                        P.i('pe', 'matmul', reads=[kr, qb], writes=[st], out=st[:, 0:nq], lhsT=kr[:, kt * 128:(kt + 1) * 128], rhs=qb[:, 0:nq], start=False, stop=True)
                        P.i('act', 'activation', reads=[st], writes=[pT], out=pT[:, 0:nq], in_=st[:, 0:nq], func=AF.Exp, scale=MLA_SCALE)
                        first, last = ii == 0, ii == len(ktiles) - 1
                        P.i('pe', 'matmul', reads=[V, pT], writes=[oacc], out=oacc[:, 0:nq], lhsT=V[:, kt, :], rhs=pT[:, 0:nq], start=first, stop=last)
                        P.i('pe', 'matmul', reads=[ones, pT], writes=[den], out=den[:, 0:nq], lhsT=ones[:], rhs=pT[:, 0:nq], start=first, stop=last)
                    o = oo.next()
                    P.i('dve', 'reciprocal', reads=[den], writes=[rden], out=rden[:, 0:nq], in_=den[:, 0:nq])
                    P.i('dve', 'tensor_tensor', reads=[oacc, rden], writes=[o], out=o[:, 0:nq], in0=oacc[:, 0:nq], in1=rden[:, 0:nq], op=ALU.mult)
                    P.i('sp', 'dma_start', reads=[o], out=T['mixT'].ap()[h * 128:(h + 1) * 128, base + t0:base + t0 + nq], in_=o[:, 0:nq])


def na_table_host(rpb):
    tab = np.full((12, 5, 128, 7, 128), -30000.0, np.float32)
    Rs = [0, 1, 30, 62, 63]
    a = np.arange(2)[:, None, None, None]
    kc = np.arange(64)[None, :, None, None]
    b_ = np.arange(2)[None, None, :, None]
    qc = np.arange(64)[None, None, None, :]
    for s, R in enumerate(Rs):
        qr = 2 * R + b_
        r0 = np.clip(qr - 4, 0, 120)
        c0 = np.clip(qc - 8, 0, 48)
        for i in range(7):
            m = R - 3 + i
            if m < 0 or m > 63:
                continue
            kr = 2 * m + a
            valid = (kr >= r0) & (kr < r0 + 8) & (kc >= c0) & (kc < c0 + 16)
            ro = np.clip(kr - qr + 7, 0, 14) + 0 * kc + 0 * qc
            co = np.clip(kc - qc + 15, 0, 30) + 0 * a + 0 * b_
            vals = rpb[:, ro, co]
            blk = np.where(valid[None], vals, np.float32(-30000.0)).reshape(12, 128, 128)
            tab[:, s, :, i, :] = blk
    return tab.reshape(12, 5, 128, 896)


def make_T3(nc, T):
    T['natab'] = din(nc, 'natab', [12, 5, 128, 896])
    T['convw'] = din(nc, 'convw', [128, 8, 3])
    T['w_out'] = din(nc, 'w_out', [D, D])
    T['mixT'] = nc.dram_tensor('mixT', [D, NTOK], BF16)
    T['h1'] = [nc.dram_tensor('h1_%d' % b, [8448, D], F32) for b in range(2)]


def phase_na(P, nc, T, batches=(0, 1), heads=range(12), qtiles=None):
    with Scope(P) as S:
        kTs = RR([S.sbuf([128, 8448], BF16) for _ in range(2)])
        qTs = RR([S.sbuf([128, 8448], BF16) for _ in range(2)])
        Vs = RR([S.sbuf([128, 66, 128], BF16) for _ in range(2)])
        tabs = RR([S.sbuf([128, 5, 896], F32) for _ in range(2)])
        ones = S.sbuf([128, 128], BF16)
        sbs = RR([S.sbuf([128, 512], F32) for _ in range(3)])
        pTs = RR([S.sbuf([128, 512], BF16) for _ in range(4)])
        rden = S.sbuf([128, 128], F32)
        oo = RR([S.sbuf([128, 128], BF16) for _ in range(3)])
        sts = RR([S.psum([128, 512], F32) for _ in range(4)])
        oaccs = RR([S.psum([128, 128], F32) for _ in range(2)])
        dens = RR([S.psum([128, 128], F32) for _ in range(2)])
        P.i('pool', 'memset', writes=[ones], ap=ones[:], constant=1.0)
        for b in batches:
            base = b * 8448
            for h in heads:
                kT, qT, V, tab = kTs.next(), qTs.next(), Vs.next(), tabs.next()
                P.i('sp', 'dma_start', writes=[kT], out=kT[:], in_=T['kcT'].ap()[h, :, base:base + 8448])
                P.i('sp', 'dma_start', writes=[qT], out=qT[:], in_=T['qcT'].ap()[h, :, base:base + 8448])
                for j4 in range(0, 66, 11):
                    P.i('sp', 'dma_start', writes=[V], out=V[:, j4:j4 + 11, :],
                        in_=T['vc'].ap()[base + j4 * 128:base + (j4 + 11) * 128, h * 128:(h + 1) * 128].rearrange("(j p) d -> p j d", p=128))
                P.i('sp', 'dma_start', writes=[tab], out=tab[:], in_=T['natab'].ap()[h].rearrange("s p f -> p s f"))
                qlist = list(range(66)) if qtiles is None else qtiles
                for R in qlist:
                    qsl = qT[:, R * 128:(R + 1) * 128]
                    groups = []
                    groups.append(([64, 65], None, 0))
                    if R < 64:
                        s = {0: 0, 1: 1, 62: 3, 63: 4}.get(R, 2)
                        iv = [i for i in range(7) if 0 <= R - 3 + i <= 63]
                        for g0 in range(0, len(iv), 4):
                            ii = iv[g0:g0 + 4]
                            groups.append(([R - 3 + i for i in ii], s, ii[0]))
                    oacc, den = oaccs.next(), dens.next()
                    nk_total = sum(len(g[0]) for g in groups)
                    done = 0
                    for (kts, s, i0) in groups:
                        st = sts.next()
                        pT = pTs.next()
                        n = len(kts) * 128
                        for x, m in enumerate(kts):
                            P.i('pe', 'matmul', reads=[kT, qT], writes=[st], out=st[:, x * 128:(x + 1) * 128], lhsT=kT[:, m * 128:(m + 1) * 128], rhs=qsl,
                                start=True, stop=True)
                        if s is None:
                            P.i('act', 'activation', reads=[st], writes=[pT], out=pT[:, 0:n], in_=st[:, 0:n], func=AF.Exp, scale=NA_SCALE)
                        else:
                            sb_ = sbs.next()
                            P.i('dve', 'scalar_tensor_tensor', reads=[st, tab], writes=[sb_], out=sb_[:, 0:n], in0=st[:, 0:n], scalar=NA_SCALE,
                                in1=tab[:, s, i0 * 128:i0 * 128 + n], op0=ALU.mult, op1=ALU.add)
                            P.i('act', 'activation', reads=[sb_], writes=[pT], out=pT[:, 0:n], in_=sb_[:, 0:n], func=AF.Exp)
                        for x, m in enumerate(kts):
                            first, last = done == 0, done == nk_total - 1
                            P.i('pe', 'matmul', reads=[V, pT], writes=[oacc], out=oacc[:], lhsT=V[:, m, :], rhs=pT[:, x * 128:(x + 1) * 128], start=first, stop=last)
                            P.i('pe', 'matmul', reads=[ones, pT], writes=[den], out=den[:], lhsT=ones[:], rhs=pT[:, x * 128:(x + 1) * 128], start=first, stop=last)
                            done += 1
                    o = oo.next()
                    P.i('dve', 'reciprocal', reads=[den], writes=[rden], out=rden[:], in_=den[:])
                    P.i('dve', 'tensor_tensor', reads=[oacc, rden], writes=[o], out=o[:], in0=oacc[:], in1=rden[:], op=ALU.mult)
                    P.i('sp', 'dma_start', reads=[o], out=T['mixT'].ap()[2560 + h * 128:2560 + (h + 1) * 128, base + R * 128:base + (R + 1) * 128], in_=o[:])


def phase_conv(P, nc, T, batches=(0, 1)):
    with Scope(P) as S:
        cw = S.sbuf([128, 8, 3], F32)
        us = RR([S.sbuf([128, 4098], F32) for _ in range(2)])
        pbs = RR([S.sbuf([128, 4096], F32) for _ in range(2)])
        acc = S.sbuf([128, 4096], F32)
        outs = RR([S.sbuf([128, 4096], BF16) for _ in range(2)])
        P.i('sp', 'dma_start', writes=[cw], out=cw[:], in_=T['convw'].ap())
        for b in batches:
            base = b * 8448
            for (t0, n, lpad, rpad) in ((0, 4096, False, True), (4096, 4096, True, False), (8192, 256, False, False)):
                for c in range(8):
                    u, pb, o = us.next(), pbs.next(), outs.next()
                    a = base + t0
                    lo = a - 1 if lpad else a
                    hi = a + n + 1 if rpad else a + n
                    P.i('pool', 'memset', writes=[u], ap=u[:, 0:n + 2], constant=0.0)
                    P.i('sp', 'dma_start', writes=[u], out=u[:, 1 - (a - lo):1 + n + (hi - a - n)], in_=T['uT'].ap()[c * 128:(c + 1) * 128, lo:hi])
                    P.i('sp', 'dma_start', writes=[pb], out=pb[:, 0:n], in_=T['pbT'].ap()[c * 128:(c + 1) * 128, a:a + n])
                    P.i('dve', 'tensor_scalar', reads=[u, cw], writes=[acc], out=acc[:, 0:n], in0=u[:, 1:n + 1], scalar1=cw[:, c, 1:2], scalar2=None, op0=ALU.mult)
                    P.i('dve', 'scalar_tensor_tensor', reads=[u, cw, acc], writes=[acc], out=acc[:, 0:n], in0=u[:, 0:n], scalar=cw[:, c, 0:1], in1=acc[:, 0:n],
                        op0=ALU.mult, op1=ALU.add)
                    P.i('dve', 'scalar_tensor_tensor', reads=[u, cw, acc], writes=[acc], out=acc[:, 0:n], in0=u[:, 2:n + 2], scalar=cw[:, c, 2:3], in1=acc[:, 0:n],
                        op0=ALU.mult, op1=ALU.add)
                    P.i('pool', 'tensor_tensor', reads=[acc, pb], writes=[o], out=o[:, 0:n], in0=acc[:, 0:n], in1=pb[:, 0:n], op=ALU.mult)
                    P.i('sp', 'dma_start', reads=[o], out=T['mixT'].ap()[1536 + c * 128:1536 + (c + 1) * 128, a:a + n], in_=o[:, 0:n])


def phase_wout(P, nc, T, tiles=None, tiles_per_group=10):
    tiles = list(range(NTILE)) if tiles is None else tiles
    wv = T['w_out'].ap().rearrange("(kc p) n -> p kc n", p=128)
    mv = T['mixT'].ap().rearrange("(kc p) t -> p kc t", p=128)
    groups = [tiles[g:g + tiles_per_group] for g in range(0, len(tiles), tiles_per_group)]
    with Scope(P) as S:
        aT = S.sbuf([128, 32, tiles_per_group * 128], BF16)
        wbs = RR([S.sbuf([128, 32, 512], BF16) for _ in range(2)])
        gates = {}
        for cond in range(3):
            gates[cond] = S.sbuf([128, D], F32)
            load_bvec(P, T, gates[cond], cond, 2)
        hcs = RR([S.sbuf([128, 512], F32) for _ in range(3)])
        ts = RR([S.sbuf([128, 512], F32) for _ in range(3)])
        accs = RR([S.psum([128, 512], F32) for _ in range(4)])
        for grp in groups:
            for li, ti in enumerate(grp):
                for k4 in range(4):
                    P.i('sp', 'dma_start', writes=[aT], out=aT[:, k4 * 8:(k4 + 1) * 8, li * 128:(li + 1) * 128], in_=mv[:, k4 * 8:(k4 + 1) * 8, ti * 128:(ti + 1) * 128])
            for ch in range(8):
                n0 = ch * 512
                wb = wbs.next()
                for s4 in range(4):
                    P.i('pool', 'dma_start', writes=[wb], out=wb[:, s4 * 8:(s4 + 1) * 8, :], in_=wv[:, s4 * 8:(s4 + 1) * 8, n0:n0 + 512])
                for li, ti in enumerate(grp):
                    b, j = divmod(ti, 66)
                    acc = accs.next()
                    hc, t = hcs.next(), ts.next()
                    P.i('sp', 'dma_start', writes=[hc], out=hc[:], in_=T['h'].ap()[ti * 128:(ti + 1) * 128, n0:n0 + 512])
                    for kc in range(32):
                        P.i('pe', 'matmul', reads=[aT, wb], writes=[acc], out=acc[:], lhsT=aT[:, kc, li * 128:(li + 1) * 128], rhs=wb[:, kc, :],
                            start=(kc == 0), stop=(kc == 31))
                    g = gates[tile_cond(ti)]
                    P.i('dve', 'tensor_tensor', reads=[acc, g], writes=[t], out=t[:], in0=acc[:], in1=g[:, n0:n0 + 512], op=ALU.mult)
                    P.i('pool', 'tensor_tensor', reads=[t, hc], writes=[t], out=t[:], in0=t[:], in1=hc[:], op=ALU.add)
                    P.i('sp', 'dma_start', reads=[t], out=T['h1'][b].ap()[j * 128:(j + 1) * 128, n0:n0 + 512], in_=t[:])


CAP_L = 1024
CAP_C = 32


def make_T4(nc, T):
    T['wrT'] = din(nc, 'wrT', [128, 32, 16])
    T['ltri'] = din(nc, 'ltri', [128, 128])
    T['ex_gate'] = din(nc, 'ex_gate', [16, D, 768])
    T['ex_up'] = din(nc, 'ex_up', [16, D, 768])
    T['ex_down'] = din(nc, 'ex_down', [16, 768, D])
    T['hn'] = nc.dram_tensor('hn', [NTOK, D], BF16)
    T['aff'] = nc.dram_tensor('aff', [NTOK, 16], F32)
    T['idxd'] = nc.dram_tensor('idxd', [128, 32, 66], I32)
    T['gmd'] = nc.dram_tensor('gmd', [128, 32, 66], F32)
    T['xe'] = [[nc.dram_tensor('xe_%d_%d' % (b, e), [CAP_L, D], BF16) for e in range(16)] for b in range(2)]
    T['xec'] = [nc.dram_tensor('xec_%d' % g, [CAP_C, D], BF16) for g in range(32)]
    T['ye'] = [[nc.dram_tensor('ye_%d_%d' % (b, e), [CAP_L, D], F32) for e in range(16)] for b in range(2)]
    T['yec'] = [nc.dram_tensor('yec_%d' % g, [CAP_C, D], F32) for g in range(32)]
    T['h_out'] = dout(nc, 'h_out', [NTOK, D])


def phase_norm2(P, nc, T, tiles=None):
    tiles = list(range(NTILE)) if tiles is None else tiles
    with Scope(P) as S:
        muls, shifts = {}, {}
        for cond in range(3):
            muls[cond] = S.sbuf([128, D], F32)
            shifts[cond] = S.sbuf([128, D], F32)
            load_bvec(P, T, muls[cond], cond, 3)
            load_bvec(P, T, shifts[cond], cond, 4)
        hts = RR([S.sbuf([128, D], F32) for _ in range(2)])
        xh = S.sbuf([128, D], F32)
        hb = RR([S.sbuf([128, D], BF16) for _ in range(2)])
        hnT = S.sbuf([128, 32, 128], F32)
        ssqs = RR([S.sbuf([128, 1], F32) for _ in range(2)])
        identf = S.sbuf([128, 128], F32)
        wr = S.sbuf([128, 32, 16], F32)
        lg = S.sbuf([128, 16], F32)
        ex = S.sbuf([128, 16], F32)
        mx = S.sbuf([128, 1], F32)
        sm = S.sbuf([128, 1], F32)
        affs = RR([S.sbuf([128, 16], F32) for _ in range(2)])
        tps = RR([S.psum([128, 512], F32) for _ in range(2)])
        lps = RR([S.psum([128, 16], F32) for _ in range(2)])
        evs = RR(['act', 'dve'])
        P.i('sp', 'dma_start', writes=[identf], out=identf[:], in_=T['identf'].ap())
        P.i('sp', 'dma_start', writes=[wr], out=wr[:], in_=T['wrT'].ap())
        for ti in tiles:
            b, j = divmod(ti, 66)
            cond = tile_cond(ti)
            ht, ssq, hbt, af = hts.next(), ssqs.next(), hb.next(), affs.next()
            P.i('sp', 'dma_start', writes=[ht], out=ht[:], in_=T['h1'][b].ap()[j * 128:(j + 1) * 128, :])
            norm_mod_tile(P, ht, xh, ssq, muls[cond], shifts[cond])
            P.i('act', 'copy', reads=[xh], writes=[hbt], out=hbt[:], in_=xh[:])
            P.i('sp', 'dma_start', reads=[hbt], out=T['hn'].ap()[ti * 128:(ti + 1) * 128, :], in_=hbt[:])
            transposes(P, tps, evs, [(xh, xh[:, kc * 128:(kc + 1) * 128], hnT, hnT[:, kc, :]) for kc in range(32)], identf)
            lp = lps.next()
            for kc in range(32):
                P.i('pe', 'matmul', reads=[hnT, wr], writes=[lp], out=lp[:], lhsT=hnT[:, kc, :], rhs=wr[:, kc, :], start=(kc == 0), stop=(kc == 31))
            P.i('dve', 'tensor_copy', reads=[lp], writes=[lg], out=lg[:], in_=lp[:])
            P.i('dve', 'tensor_reduce', reads=[lg], writes=[mx], out=mx[:], in_=lg[:], axis=AX.X, op=ALU.max)
            P.i('dve', 'tensor_scalar', reads=[mx], writes=[mx], out=mx[:], in0=mx[:], scalar1=-1.0, scalar2=None, op0=ALU.mult)
            P.i('act', 'activation', reads=[lg, mx], writes=[ex], out=ex[:], in_=lg[:], func=AF.Exp, bias=mx[:, 0:1])
            P.i('dve', 'tensor_reduce', reads=[ex], writes=[sm], out=sm[:], in_=ex[:], axis=AX.X, op=ALU.add)
            P.i('dve', 'reciprocal', reads=[sm], writes=[sm], out=sm[:], in_=sm[:])
            P.i('dve', 'tensor_scalar', reads=[ex, sm], writes=[af], out=af[:], in0=ex[:], scalar1=sm[:, 0:1], scalar2=None, op0=ALU.mult)
            P.i('sp', 'dma_start', reads=[af], out=T['aff'].ap()[ti * 128:(ti + 1) * 128, :], in_=af[:])


def bisect_topk(P, S, aff, G, J, cap, onesf, cps, niter=40):
    lo = S.sbuf([128, G], F32)
    hi = S.sbuf([128, G], F32)
    mid = S.sbuf([128, G], F32)
    m = S.sbuf([128, G], F32)
    t = S.sbuf([128, G], F32)
    cntp = S.sbuf([128, G], F32)
    cmp_ = S.sbuf([128, G, J], F32)
    P.i('pool', 'memset', writes=[lo], ap=lo[:], constant=0.0)
    P.i('pool', 'memset', writes=[hi], ap=hi[:], constant=1.0)
    for it in range(niter):
        P.i('dve', 'tensor_tensor', reads=[lo, hi], writes=[mid], out=mid[:], in0=lo[:], in1=hi[:], op=ALU.add)
        P.i('dve', 'tensor_scalar', reads=[mid], writes=[mid], out=mid[:], in0=mid[:], scalar1=0.5, scalar2=None, op0=ALU.mult)
        P.i('dve', 'tensor_tensor', reads=[aff, mid], writes=[cmp_], out=cmp_[:], in0=aff[:], in1=mid[:].unsqueeze(2).broadcast_to([128, G, J]), op=ALU.is_ge)
        P.i('dve', 'tensor_reduce', reads=[cmp_], writes=[cntp], out=cntp[:], in_=cmp_[:], axis=AX.X, op=ALU.add)
        cp = cps.next()
        P.i('pe', 'matmul', reads=[onesf, cntp], writes=[cp], out=cp[:], lhsT=onesf[:], rhs=cntp[:], start=True, stop=True)
        P.i('dve', 'tensor_scalar', reads=[cp], writes=[m], out=m[:], in0=cp[:], scalar1=cap - 0.5, scalar2=None, op0=ALU.is_ge)
        P.i('dve', 'tensor_tensor', reads=[mid, m], writes=[t], out=t[:], in0=mid[:], in1=m[:], op=ALU.mult)
        P.i('dve', 'tensor_tensor', reads=[lo, t], writes=[lo], out=lo[:], in0=lo[:], in1=t[:], op=ALU.max)
        P.i('dve', 'scalar_tensor_tensor', reads=[m, mid], writes=[t], out=t[:], in0=m[:], scalar=10.0, in1=mid[:], op0=ALU.mult, op1=ALU.add)
        P.i('dve', 'tensor_tensor', reads=[hi, t], writes=[hi], out=hi[:], in0=hi[:], in1=t[:], op=ALU.min)
    P.i('dve', 'tensor_tensor', reads=[aff, lo], writes=[cmp_], out=cmp_[:], in0=aff[:], in1=lo[:].unsqueeze(2).broadcast_to([128, G, J]), op=ALU.is_ge)
    return cmp_, lo


def ranks(P, S, mask, G, J, ltri, onesf, offset, pss):
    N = G * J
    r1 = S.sbuf([128, G, J], F32)
    cs = S.sbuf([128, G, J], F32)
    s1 = S.sbuf([128, G, J], F32)
    mflat = mask[:].rearrange("p g j -> p (g j)")
    r1f = r1[:].rearrange("p g j -> p (g j)")
    csf = cs[:].rearrange("p g j -> p (g j)")
    for n0 in range(0, N, 512):
        n = min(512, N - n0)
        ps = pss.next()
        P.i('pe', 'matmul', reads=[ltri, mask], writes=[ps], out=ps[:, 0:n], lhsT=ltri[:], rhs=mflat[:, n0:n0 + n], start=True, stop=True)
        P.i('dve', 'tensor_copy', reads=[ps], writes=[r1], out=r1f[:, n0:n0 + n], in_=ps[:, 0:n])
        ps = pss.next()
        P.i('pe', 'matmul', reads=[onesf, mask], writes=[ps], out=ps[:, 0:n], lhsT=onesf[:], rhs=mflat[:, n0:n0 + n], start=True, stop=True)
        P.i('dve', 'tensor_copy', reads=[ps], writes=[cs], out=csf[:, n0:n0 + n], in_=ps[:, 0:n])
    P.i('dve', 'tensor_tensor', reads=[r1, cs], writes=[r1], out=r1[:], in0=r1[:], in1=cs[:], op=ALU.subtract)
    a, b_ = cs, s1
    d = 1
    while d < J:
        P.i('dve', 'tensor_copy', reads=[a], writes=[b_], out=b_[:, :, 0:d], in_=a[:, :, 0:d])
        P.i('dve', 'tensor_tensor', reads=[a], writes=[b_], out=b_[:, :, d:J], in0=a[:, :, d:J], in1=a[:, :, 0:J - d], op=ALU.add)
        a, b_ = b_, a
        d *= 2
    P.i('dve', 'tensor_tensor', reads=[r1, a], writes=[r1], out=r1[:], in0=r1[:], in1=a[:], op=ALU.add)
    P.i('dve', 'tensor_scalar', reads=[r1], writes=[r1], out=r1[:], in0=r1[:], scalar1=float(offset) - 4096.0, scalar2=None, op0=ALU.add)
    P.i('dve', 'tensor_tensor', reads=[r1, mask], writes=[r1], out=r1[:], in0=r1[:], in1=mask[:], op=ALU.mult)
    P.i('dve', 'tensor_scalar', reads=[r1], writes=[r1], out=r1[:], in0=r1[:], scalar1=4096.0, scalar2=None, op0=ALU.add)
    return r1


def phase_topk(P, nc, T):
    with Scope(P) as S:
        affJ = S.sbuf([128, 2, 66, 16], F32)
        affL = S.sbuf([128, 32, 64], F32)
        affC = S.sbuf([128, 32, 2], F32)
        onesf = S.sbuf([128, 128], F32)
        ltri = S.sbuf([128, 128], F32)
        idxf = S.sbuf([128, 32, 66], F32)
        idxi = S.sbuf([128, 32, 66], I32)
        gm = S.sbuf([128, 32, 66], F32)
        P.i('pool', 'memset', writes=[onesf], ap=onesf[:], constant=1.0)
        P.i('sp', 'dma_start', writes=[ltri], out=ltri[:], in_=T['ltri'].ap())
        for b in range(2):
            P.i('sp', 'dma_start', writes=[affJ], out=affJ[:, b, :, :], in_=T['aff'].ap()[b * 8448:(b + 1) * 8448, :].rearrange("(j p) e -> p j e", p=128))
            P.i('dve', 'tensor_copy', reads=[affJ], writes=[affL], out=affL[:, b * 16:(b + 1) * 16, :], in_=affJ[:, b, 0:64, :].rearrange("p j e -> p e j"))
            P.i('dve', 'tensor_copy', reads=[affJ], writes=[affC], out=affC[:, b * 16:(b + 1) * 16, :], in_=affJ[:, b, 64:66, :].rearrange("p j e -> p e j"))
        cps = RR([S.psum([128, 32], F32) for _ in range(2)])
        pss = RR([S.psum([128, 512], F32) for _ in range(2)])
        maskL, _ = bisect_topk(P, S, affL, 32, 64, CAP_L, onesf, cps)
        maskC, _ = bisect_topk(P, S, affC, 32, 2, CAP_C, onesf, cps)
        rL = ranks(P, S, maskL, 32, 64, ltri, onesf, 0, pss)
        rC = ranks(P, S, maskC, 32, 2, ltri, onesf, 0, pss)
        P.i('dve', 'tensor_copy', reads=[rL], writes=[idxf], out=idxf[:, :, 0:64], in_=rL[:])
        P.i('dve', 'tensor_copy', reads=[rC], writes=[idxf], out=idxf[:, :, 64:66], in_=rC[:])
        P.i('dve', 'tensor_copy', reads=[idxf], writes=[idxi], out=idxi[:], in_=idxf[:])
        P.i('dve', 'tensor_tensor', reads=[maskL, affL], writes=[gm], out=gm[:, :, 0:64], in0=maskL[:], in1=affL[:], op=ALU.mult)
        P.i('dve', 'tensor_tensor', reads=[maskC, affC], writes=[gm], out=gm[:, :, 64:66], in0=maskC[:], in1=affC[:], op=ALU.mult)
        P.i('sp', 'dma_start', reads=[idxi], out=T['idxd'].ap(), in_=idxi[:])
        P.i('sp', 'dma_start', reads=[gm], out=T['gmd'].ap(), in_=gm[:])


def phase_scatter(P, nc, T):
    with Scope(P) as S:
        idx = S.sbuf([128, 32, 66], I32)
        hts = RR([S.sbuf([128, D], BF16) for _ in range(3)])
        P.i('sp', 'dma_start', writes=[idx], out=idx[:], in_=T['idxd'].ap())
        for ti in range(NTILE):
            b, j = divmod(ti, 66)
            ht = hts.next()
            P.i('sp', 'dma_start', writes=[ht], out=ht[:], in_=T['hn'].ap()[ti * 128:(ti + 1) * 128, :])
            for e in range(16):
                g = b * 16 + e
                if j < 64:
                    dst, bc = T['xe'][b][e].ap(), CAP_L - 1
                else:
                    dst, bc = T['xec'][g].ap(), CAP_C - 1
                P.i('pool', 'indirect_dma_start', reads=[ht, idx], out=dst, out_offset=bass.IndirectOffsetOnAxis(ap=idx[:, g, j:j + 1], axis=0),
                    in_=ht[:], in_offset=None, bounds_check=bc, oob_is_err=False)


def phase_ffn(P, nc, T, pairs=None):
    pairs = [(b, e) for b in range(2) for e in range(16)] if pairs is None else pairs
    NR = CAP_L + CAP_C
    with Scope(P) as S:
        identb = S.sbuf([128, 128], BF16)
        xeT = S.sbuf([128, 32, NR], BF16)
        HT = S.sbuf([128, 6, NR], BF16)
        xts = RR([S.sbuf([128, D], BF16) for _ in range(2)])
        wgs = RR([S.sbuf([128, 32, 256], BF16) for _ in range(2)])
        wus = RR([S.sbuf([128, 32, 256], BF16) for _ in range(2)])
        wds = RR([S.sbuf([128, 6, 512], BF16) for _ in range(2)])
        sgs = RR([S.sbuf([128, 512], F32) for _ in range(2)])
        ys = RR([S.sbuf([128, 512], F32) for _ in range(3)])
        tpb = RR([S.psum([128, 512], BF16) for _ in range(2)])
        gas = RR([S.psum([128, 512], F32) for _ in range(2)])
        gus = RR([S.psum([128, 512], F32) for _ in range(2)])
        yps = RR([S.psum([128, 512], F32) for _ in range(2)])
        evs = RR(['act', 'dve'])
        P.i('sp', 'dma_start', writes=[identb], out=identb[:], in_=T['identb'].ap())
        for (b, e) in pairs:
            g = b * 16 + e
            for rt in range(9):
                xt = xts.next()
                nr = 128 if rt < 8 else CAP_C
                if rt < 8:
                    P.i('sp', 'dma_start', writes=[xt], out=xt[:], in_=T['xe'][b][e].ap()[rt * 128:(rt + 1) * 128, :])
                else:
                    P.i('sp', 'dma_start', writes=[xt], out=xt[0:CAP_C, :], in_=T['xec'][g].ap())
                blocks = [(xt, xt[0:nr, kc * 128:(kc + 1) * 128], xeT, xeT[:, kc, rt * 128:rt * 128 + nr]) for kc in range(32)]
                for g0 in range(0, 32, 4):
                    tp = tpb.next()
                    for q in range(4):
                        sb_, sap, db_, dap = blocks[g0 + q]
                        P.i('pe', 'transpose', reads=[xt, identb], writes=[tp], out=tp[:, q * 128:q * 128 + nr], in_=sap, identity=identb[0:nr, 0:nr])
                    for q in range(4):
                        sb_, sap, db_, dap = blocks[g0 + q]
                        if evs.next() == 'act':
                            P.i('act', 'copy', reads=[tp], writes=[xeT], out=dap, in_=tp[:, q * 128:q * 128 + nr])
                        else:
                            P.i('dve', 'tensor_copy', reads=[tp], writes=[xeT], out=dap, in_=tp[:, q * 128:q * 128 + nr])
            gv = T['ex_gate'].ap()[e].rearrange("(kc p) f -> p kc f", p=128)
            uv = T['ex_up'].ap()[e].rearrange("(kc p) f -> p kc f", p=128)
            for half in range(3):
                wg, wu = wgs.next(), wus.next()
                for s4 in range(4):
                    P.i('pool', 'dma_start', writes=[wg], out=wg[:, s4 * 8:(s4 + 1) * 8, :], in_=gv[:, s4 * 8:(s4 + 1) * 8, half * 256:(half + 1) * 256])
                    P.i('pool', 'dma_start', writes=[wu], out=wu[:, s4 * 8:(s4 + 1) * 8, :], in_=uv[:, s4 * 8:(s4 + 1) * 8, half * 256:(half + 1) * 256])
                for f3 in range(2):
                    fc = half * 2 + f3
                    for (t0, n) in ((0, 512), (512, 512), (1024, CAP_C)):
                        ga, gu = gas.next(), gus.next()
                        for kc in range(32):
                            P.i('pe', 'matmul', reads=[wg, xeT], writes=[ga], out=ga[:, 0:n], lhsT=wg[:, kc, f3 * 128:(f3 + 1) * 128], rhs=xeT[:, kc, t0:t0 + n],
                                start=(kc == 0), stop=(kc == 31))
                        for kc in range(32):
                            P.i('pe', 'matmul', reads=[wu, xeT], writes=[gu], out=gu[:, 0:n], lhsT=wu[:, kc, f3 * 128:(f3 + 1) * 128], rhs=xeT[:, kc, t0:t0 + n],
                                start=(kc == 0), stop=(kc == 31))
                        sg = sgs.next()
                        P.i('act', 'activation', reads=[ga], writes=[sg], out=sg[:, 0:n], in_=ga[:, 0:n], func=AF.Silu)
                        P.i('dve', 'tensor_tensor', reads=[sg, gu], writes=[HT], out=HT[:, fc, t0:t0 + n], in0=sg[:, 0:n], in1=gu[:, 0:n], op=ALU.mult)
            dv = T['ex_down'].ap()[e].rearrange("(fc p) d -> p fc d", p=128)
            for dc in range(8):
                wd = wds.next()
                P.i('pool', 'dma_start', writes=[wd], out=wd[:], in_=dv[:, :, dc * 512:(dc + 1) * 512])
                for rt in range(9):
                    nr = 128 if rt < 8 else CAP_C
                    yp = yps.next()
                    for fc in range(6):
                        P.i('pe', 'matmul', reads=[HT, wd], writes=[yp], out=yp[0:nr, :], lhsT=HT[:, fc, rt * 128:rt * 128 + nr], rhs=wd[:, fc, :],
                            start=(fc == 0), stop=(fc == 5))
                    y = ys.next()
                    if evs.next() == 'act':
                        P.i('act', 'copy', reads=[yp], writes=[y], out=y[0:nr, :], in_=yp[0:nr, :])
                    else:
                        P.i('dve', 'tensor_copy', reads=[yp], writes=[y], out=y[0:nr, :], in_=yp[0:nr, :])
                    if rt < 8:
                        dst = T['ye'][b][e].ap()[rt * 128:(rt + 1) * 128, dc * 512:(dc + 1) * 512]
                    else:
                        dst = T['yec'][g].ap()[:, dc * 512:(dc + 1) * 512]
                    P.i('sp', 'dma_start', reads=[y], out=dst, in_=y[0:nr, :])


def phase_combine(P, nc, T, tiles=None):
    tiles = list(range(NTILE)) if tiles is None else tiles
    with Scope(P) as S:
        idx = S.sbuf([128, 32, 66], I32)
        gm = S.sbuf([128, 32, 66], F32)
        gates = {}
        for cond in range(3):
            gates[cond] = S.sbuf([128, D], F32)
            load_bvec(P, T, gates[cond], cond, 5)
        gs = RR([S.sbuf([128, D], F32) for _ in range(3)])
        acc = S.sbuf([128, D], F32)
        hts = RR([S.sbuf([128, D], F32) for _ in range(2)])
        P.i('sp', 'dma_start', writes=[idx], out=idx[:], in_=T['idxd'].ap())
        P.i('sp', 'dma_start', writes=[gm], out=gm[:], in_=T['gmd'].ap())
        for gb in gs.items:
            P.i('pool', 'memset', writes=[gb], ap=gb[:], constant=0.0)
        for ti in tiles:
            b, j = divmod(ti, 66)
            ht = hts.next()
            P.i('sp', 'dma_start', writes=[ht], out=ht[:], in_=T['h1'][b].ap()[j * 128:(j + 1) * 128, :])
            for e in range(16):
                g = b * 16 + e
                gt = gs.next()
                if j < 64:
                    src, bc = T['ye'][b][e].ap(), CAP_L - 1
                else:
                    src, bc = T['yec'][g].ap(), CAP_C - 1
                P.i('pool', 'indirect_dma_start', reads=[idx], writes=[gt], out=gt[:], out_offset=None, in_=src,
                    in_offset=bass.IndirectOffsetOnAxis(ap=idx[:, g, j:j + 1], axis=0), bounds_check=bc, oob_is_err=False)
                if e == 0:
                    P.i('dve', 'tensor_scalar', reads=[gt, gm], writes=[acc], out=acc[:], in0=gt[:], scalar1=gm[:, g, j:j + 1], scalar2=None, op0=ALU.mult)
                else:
                    P.i('dve', 'scalar_tensor_tensor', reads=[gt, gm, acc], writes=[acc], out=acc[:], in0=gt[:], scalar=gm[:, g, j:j + 1], in1=acc[:],
                        op0=ALU.mult, op1=ALU.add)
            gate = gates[tile_cond(ti)]
            P.i('dve', 'tensor_tensor', reads=[acc, gate], writes=[acc], out=acc[:], in0=acc[:], in1=gate[:], op=ALU.mult)
            P.i('pool', 'tensor_tensor', reads=[acc, ht], writes=[ht], out=ht[:], in0=acc[:], in1=ht[:], op=ALU.add)
            P.i('sp', 'dma_start', reads=[ht], out=T['h_out'].ap()[ti * 128:(ti + 1) * 128, :], in_=ht[:])


def build_layer(dbg_h1=False):
    nc = bass.Bass("TRN2", target_bir_lowering=False)
    T = make_T(nc)
    make_T2(nc, T)
    make_T3(nc, T)
    make_T4(nc, T)
    P = Prog(nc)
    phase_ada(P, nc, T)
    phase_win(P, nc, T)
    phase_prep(P, nc, T)
    phase_mla(P, nc, T)
    phase_na(P, nc, T)
    phase_conv(P, nc, T)
    phase_wout(P, nc, T)
    phase_norm2(P, nc, T)
    phase_topk(P, nc, T)
    phase_scatter(P, nc, T)
    phase_ffn(P, nc, T)
    phase_combine(P, nc, T)
    if dbg_h1:
        d1 = dout(nc, 'd_h1', [NTOK, D])
        d2 = dout(nc, 'd_aff', [NTOK, 16])
        d3 = dout(nc, 'd_idx', [128, 32, 66], I32)
        with Scope(P) as S:
            for b in range(2):
                P.i('sp', 'dma_start', out=d1.ap()[b * 8448:(b + 1) * 8448, :], in_=T['h1'][b].ap())
            P.i('sp', 'dma_start', out=d2.ap(), in_=T['aff'].ap())
            P.i('sp', 'dma_start', out=d3.ap(), in_=T['idxd'].ap())
    P.close()
    return nc


_NC_CACHE = {}


def _rep(v, n=128):
    v = np.asarray(v)
    return np.ascontiguousarray(np.broadcast_to(v.reshape(1, -1), (n, v.size))).astype(np.float32)


def _rope_table():
    pos = np.arange(8192)
    row = (pos // 64).astype(np.float32)
    col = (pos % 64).astype(np.float32)
    inv = np.power(np.float32(10000.0), -np.arange(16, dtype=np.float32) / 16).astype(np.float32)
    cs1 = np.concatenate([np.cos(row[:, None] * inv), np.sin(row[:, None] * inv),
                          np.cos(col[:, None] * inv), np.sin(col[:, None] * inv)], 1).astype(np.float32)
    csc = np.tile(np.concatenate([np.ones(16), np.zeros(16), np.ones(16), np.zeros(16)]).astype(np.float32), (256, 1))
    return np.concatenate([cs1, csc, cs1, csc], 0)


def kernel(x, c, ctx, c_ctx, ada_down, ada_up, ada_bias, norm1_g, w_in, mla_qa_g, mla_kva_g,
           mla_w_uq, mla_w_ukv, mla_q_g, mla_k_g, conv_w, na_q_g, na_k_g, na_rpb, w_out,
           norm2_g, w_router, ex_gate, ex_up, ex_down):
    x = np.asarray(x, np.float32)
    ctx = np.asarray(ctx, np.float32)
    h = np.concatenate([x[0], ctx[0], x[1], ctx[1]], 0)
    conds = np.stack([np.asarray(c)[0], np.asarray(c)[1], np.asarray(c_ctx)]).astype(np.float32)
    cT = np.ascontiguousarray(conds.reshape(3, 32, 128).transpose(2, 1, 0))
    cs = _rope_table()
    identf = np.eye(128, dtype=np.float32)
    identb = np.eye(128).astype(ml_dtypes.bfloat16)
    ltri = np.triu(np.ones((128, 128), np.float32), 1)
    if 'nc' not in _NC_CACHE:
        _NC_CACHE['nc'] = build_layer()
    nc = _NC_CACHE['nc']
    for l in range(4):
        ins = {"h": h, "cT": cT, "ada_down": ada_down[l], "ada_up": ada_up[l],
               "bias3": _rep(ada_bias[l], 3), "g1_3": _rep(norm1_g[l], 3), "g2_3": _rep(norm2_g[l], 3),
               "w_in": w_in[l], "identf": identf, "identb": identb,
               "w_uq": mla_w_uq[l], "w_ukv": mla_w_ukv[l], "gqa": _rep(mla_qa_g[l]), "gkva": _rep(mla_kva_g[l]),
               "gq12": _rep(np.tile(mla_q_g[l], 12)), "gk12": _rep(np.tile(mla_k_g[l], 12)),
               "gnq": _rep(np.tile(na_q_g[l], 12)), "gnk": _rep(np.tile(na_k_g[l], 12)), "cs": cs,
               "natab": na_table_host(np.asarray(na_rpb[l], np.float32)),
               "convw": np.ascontiguousarray(np.asarray(conv_w[l]).reshape(3, 8, 128).transpose(2, 1, 0)),
               "w_out": w_out[l],
               "wrT": np.ascontiguousarray(np.asarray(w_router[l]).reshape(32, 128, 16).transpose(1, 0, 2)), "ltri": ltri,
               "ex_gate": ex_gate[l], "ex_up": ex_up[l], "ex_down": ex_down[l]}
        ins = {k: np.ascontiguousarray(np.asarray(v)) for k, v in ins.items()}
        res = run_bass_kernel_spmd(nc, [ins], core_ids=[0])
        h = np.asarray(res.results[0]["h_out"], np.float32)
    out = np.stack([h[0:8192], h[8448:8448 + 8192]], 0)
    return np.ascontiguousarray(out.astype(np.float32))
```
